# Optimizing a Trainium2 kernel written in Bass

```python
import numpy as np
import jax
import jax.numpy as jnp
from jax import lax

D_MODEL = 2048
BATCH = 2
SEQ = 8192
DEPTH = 2

HEAD_DIM = 64
MIX_WIDTH = D_MODEL
A_WIDTH = MIX_WIDTH // 4
A_HEADS = A_WIDTH // HEAD_DIM
A_KV_HEADS = 2
CMP_BLOCK = 32
CMP_STRIDE = 16
SEL_BLOCK = 64
SEL_TOPK = 16
SEL_LOCAL = 2
NSA_WINDOW = 512
Q_CHUNK = 128
FORCE_BONUS = 1.0e4
B_WIDTH = MIX_WIDTH // 2
B_HEADS = 4
B_HEAD_DIM = B_WIDTH // B_HEADS
CONV_WIDTH = 4
MLSTM_CHUNK = 64
C_WIDTH = MIX_WIDTH - A_WIDTH - B_WIDTH
C_HEADS = C_WIDTH // HEAD_DIM
C_KV_HEADS = 2
SWA_WINDOW = 128
BAND_BLOCK = 128
NEG_INF = -1.0e30
EPS = 1.0e-6
IN_SPLITS = (
    A_WIDTH,
    A_KV_HEADS * HEAD_DIM,
    A_KV_HEADS * HEAD_DIM,
    A_KV_HEADS * HEAD_DIM,
    A_KV_HEADS * HEAD_DIM,
    A_KV_HEADS * HEAD_DIM,
    A_KV_HEADS * HEAD_DIM,
    A_HEADS * 3,
    A_WIDTH,
    2 * B_WIDTH,
    B_WIDTH,
    B_HEADS,
    B_HEADS,
    B_WIDTH,
    B_WIDTH,
    C_WIDTH,
    C_KV_HEADS * HEAD_DIM,
    C_KV_HEADS * HEAD_DIM,
    C_WIDTH,
)
IN_COLS = sum(IN_SPLITS)

kernel_name = 'hymba_nsa_mlstm_swa_hybrid'


def rmsnorm(x, g):
    xf = x.astype(jnp.float32)
    y = xf * lax.rsqrt(jnp.mean(xf * xf, axis=-1, keepdims=True) + EPS)
    return (y * g.astype(jnp.float32)).astype(x.dtype)


def alibi_slopes(n_heads):
    return 2.0 ** (-8.0 * jnp.arange(1, n_heads + 1, dtype=jnp.float32) / n_heads)


def causal_depthwise_conv(x, w, b):
    T = x.shape[1]
    xp = jnp.pad(x, ((0, 0), (w.shape[0] - 1, 0), (0, 0)))
    y = b
    for i in range(w.shape[0]):
        y = y + xp[:, i:i + T] * w[i]
    return y


def banded_attention(q, k, v, slopes, window, sinks):
    bsz, T, H, d = q.shape
    G = k.shape[2]
    hpg = H // G
    blk = BAND_BLOCK
    nblk = T // blk
    span = blk + window
    scale = d ** -0.5
    kp = jnp.pad(k, ((0, 0), (window, 0), (0, 0), (0, 0)))
    vp = jnp.pad(v, ((0, 0), (window, 0), (0, 0), (0, 0)))
    qb = q.reshape(bsz, nblk, blk, G, hpg, d).transpose(1, 0, 2, 3, 4, 5)
    key_off = jnp.arange(span)[None, :]
    dist = (jnp.arange(blk)[:, None] + window) - key_off
    band = (dist >= 0) & (dist < window)
    slope_term = slopes.reshape(G, hpg)[:, :, None, None] * dist.astype(jnp.float32)

    def one_block(args):
        qj, j = args
        kj = lax.dynamic_slice_in_dim(kp, j * blk, span, axis=1)
        vj = lax.dynamic_slice_in_dim(vp, j * blk, span, axis=1)
        sc = jnp.einsum('bqghd,bsgd->bghqs', qj, kj).astype(jnp.float32) * scale - slope_term
        mask = band & (key_off >= window - j * blk)
        sc = jnp.where(mask, sc, NEG_INF)
        if sinks is None:
            p = jax.nn.softmax(sc, axis=-1)
        else:
            sk = sinks.astype(jnp.float32).reshape(G, hpg)[:, :, None, None]
            mx = jnp.maximum(sc.max(axis=-1, keepdims=True), sk)
            e = jnp.exp(sc - mx)
            p = e / (e.sum(axis=-1, keepdims=True) + jnp.exp(sk - mx))
        return jnp.einsum('bghqs,bsgd->bqghd', p.astype(v.dtype), vj)

    o = lax.map(one_block, (qb, jnp.arange(nblk)))
    return o.transpose(1, 0, 2, 3, 4, 5).reshape(bsz, T, H, d)


def nsa_compress(k, pe, w1, w2):
    bsz, T, G, d = k.shape
    n_cmp = (T - CMP_BLOCK) // CMP_STRIDE + 1
    idx = jnp.arange(n_cmp)[:, None] * CMP_STRIDE + jnp.arange(CMP_BLOCK)[None, :]
    blocks = k[:, idx] + pe[None, None, :, None, :]
    blocks = blocks.transpose(0, 1, 3, 2, 4).reshape(bsz, n_cmp, G, CMP_BLOCK * d)
    return jax.nn.silu(blocks @ w1) @ w2


def nsa_compressed_and_selected(q, k_cmp, v_cmp, k_slc, v_slc, slopes):
    bsz, T, H, d = q.shape
    G = k_cmp.shape[2]
    hpg = H // G
    n_cmp = k_cmp.shape[1]
    n_sel = T // SEL_BLOCK
    n_top = min(SEL_TOPK, n_sel)
    nq = T // Q_CHUNK
    scale = d ** -0.5
    cmp_start = jnp.arange(n_cmp) * CMP_STRIDE
    cmp_end = cmp_start + CMP_BLOCK - 1
    sel_start = jnp.arange(n_sel) * SEL_BLOCK
    overlap = ((cmp_start[:, None] < sel_start[None, :] + SEL_BLOCK)
               & (cmp_start[:, None] + CMP_BLOCK > sel_start[None, :])).astype(jnp.float32)
    kb = k_slc.reshape(bsz, n_sel, SEL_BLOCK, G, d).transpose(0, 3, 1, 2, 4)
    vb = v_slc.reshape(bsz, n_sel, SEL_BLOCK, G, d).transpose(0, 3, 1, 2, 4)
    qc = q.reshape(bsz, nq, Q_CHUNK, G, hpg, d).transpose(1, 0, 2, 3, 4, 5)
    sl = slopes.reshape(G, hpg)[None, :, :, None, None]
    b_idx = jnp.arange(bsz)[:, None, None, None]
    g_idx = jnp.arange(G)[None, :, None, None]
    r_idx = jnp.arange(Q_CHUNK)[None, None, :, None]
    sel_ids = jnp.arange(n_sel)[None, :]

    def one_chunk(args):
        qi, c = args
        t = c * Q_CHUNK + jnp.arange(Q_CHUNK)
        s_cmp = jnp.einsum('bqghd,bigd->bghqi', qi, k_cmp).astype(jnp.float32) * scale
        vis = cmp_end[None, :] <= t[:, None]
        p_cmp = jax.nn.softmax(jnp.where(vis, s_cmp, NEG_INF), axis=-1)
        p_cmp = p_cmp * vis.any(axis=-1)[:, None].astype(jnp.float32)
        o_cmp = jnp.einsum('bghqi,bigd->bqghd', p_cmp.astype(v_cmp.dtype), v_cmp)
        imp = jnp.einsum('bghqi,ij->bgqj', p_cmp, overlap)
        cur = (t // SEL_BLOCK)[:, None]
        valid = sel_ids <= cur
        forced = (sel_ids == 0) | (valid & (sel_ids > cur - SEL_LOCAL))
        imp = jnp.where(forced, FORCE_BONUS, jnp.where(valid, imp, -1.0))
        _, top = lax.top_k(imp, n_top)
        chosen = valid[r_idx, top]
        ks = kb[b_idx, g_idx, top].reshape(bsz, G, Q_CHUNK, n_top * SEL_BLOCK, d)
        vs = vb[b_idx, g_idx, top].reshape(bsz, G, Q_CHUNK, n_top * SEL_BLOCK, d)
        pos = (top[..., None] * SEL_BLOCK + jnp.arange(SEL_BLOCK)).reshape(bsz, G, Q_CHUNK, n_top * SEL_BLOCK)
        dist = t[None, None, :, None] - pos
        mask = jnp.repeat(chosen, SEL_BLOCK, axis=-1) & (dist >= 0)
        s_slc = (jnp.einsum('bqghd,bgqsd->bghqs', qi, ks).astype(jnp.float32) * scale
                 - sl * dist[:, :, None].astype(jnp.float32))
        p_slc = jax.nn.softmax(jnp.where(mask[:, :, None], s_slc, NEG_INF), axis=-1)
        o_slc = jnp.einsum('bghqs,bgqsd->bqghd', p_slc.astype(vs.dtype), vs)
        return o_cmp, o_slc

    o_cmp, o_slc = lax.map(one_chunk, (qc, jnp.arange(nq)))
    o_cmp = o_cmp.transpose(1, 0, 2, 3, 4, 5).reshape(bsz, T, H, d)
    o_slc = o_slc.transpose(1, 0, 2, 3, 4, 5).reshape(bsz, T, H, d)
    return o_cmp, o_slc


def mlstm_chunkwise(q, k, v, i_pre, f_pre):
    bsz, T, H, d = q.shape
    L = MLSTM_CHUNK
    nC = T // L

    def chunks(a):
        return a.reshape(bsz, nC, L, H, -1).transpose(1, 0, 3, 2, 4)

    qc, kc, vc = chunks(q), chunks(k), chunks(v)
    ic = i_pre.reshape(bsz, nC, L, H).transpose(1, 0, 3, 2)
    lfc = jax.nn.log_sigmoid(f_pre).reshape(bsz, nC, L, H).transpose(1, 0, 3, 2)
    causal = jnp.tril(jnp.ones((L, L), dtype=bool))

    def step(carry, xs):
        C, n, m = carry
        q_, k_, v_, i_, lf_ = xs
        b = jnp.cumsum(lf_, axis=-1)
        b_last = b[..., -1]
        log_d = jnp.where(causal, b[..., :, None] - b[..., None, :] + i_[..., None, :], -jnp.inf)
        log_inter = b + m[..., None]
        m_t = jnp.maximum(log_inter, log_d.max(axis=-1))
        w_intra = jnp.exp(log_d - m_t[..., None])
        w_inter = jnp.exp(log_inter - m_t)
        qk = jnp.einsum('bhtd,bhsd->bhts', q_, k_) * w_intra
        num = (w_inter[..., None] * jnp.einsum('bhtd,bhde->bhte', q_, C)
               + jnp.einsum('bhts,bhse->bhte', qk, v_))
        den = w_inter * jnp.einsum('bhtd,bhd->bht', q_, n) + qk.sum(axis=-1)
        h = num / jnp.maximum(jnp.abs(den), jnp.exp(-m_t))[..., None]
        log_g = b_last[..., None] - b + i_
        m_new = jnp.maximum(b_last + m, log_g.max(axis=-1))
        w_g = jnp.exp(log_g - m_new[..., None])
        decay = jnp.exp(b_last + m - m_new)
        C = decay[..., None, None] * C + jnp.einsum('bhsd,bhse->bhde', k_ * w_g[..., None], v_)
        n = decay[..., None] * n + jnp.einsum('bhs,bhsd->bhd', w_g, k_)
        return (C, n, m_new), h

    init = (jnp.zeros((bsz, H, d, v.shape[-1]), jnp.float32),
            jnp.zeros((bsz, H, d), jnp.float32),
            jnp.zeros((bsz, H), jnp.float32))
    _, h = lax.scan(step, init, (qc, kc, vc, ic, lfc))
    return h.transpose(1, 0, 3, 2, 4).reshape(bsz, T, H, -1)


def hybrid_layer(x, norm_g, w_in, w_out, cmp_pe_k, cmp_w1_k, cmp_w2_k, cmp_pe_v, cmp_w1_v, cmp_w2_v,
                 conv_w, conv_b, i_bias, f_bias, mnorm_g, sinks):
    bsz, T, _ = x.shape
    f32 = jnp.float32
    h = rmsnorm(x, norm_g)
    proj = h @ w_in
    split_at = [int(p) for p in np.cumsum(IN_SPLITS)[:-1]]
    (a_q, a_kc, a_vc, a_ks, a_vs, a_kw, a_vw, a_gate, a_z,
     b_qk, b_v, b_i, b_f, b_o, b_z,
     c_q, c_k, c_v, c_z) = jnp.split(proj, split_at, axis=-1)

    def heads(a, n):
        return a.reshape(bsz, T, n, -1)

    slopes_a = alibi_slopes(A_HEADS)
    q_a = heads(a_q, A_HEADS)
    k_cmp = nsa_compress(heads(a_kc, A_KV_HEADS), cmp_pe_k, cmp_w1_k, cmp_w2_k)
    v_cmp = nsa_compress(heads(a_vc, A_KV_HEADS), cmp_pe_v, cmp_w1_v, cmp_w2_v)
    o_cmp, o_slc = nsa_compressed_and_selected(q_a, k_cmp, v_cmp, heads(a_ks, A_KV_HEADS),
                                               heads(a_vs, A_KV_HEADS), slopes_a)
    o_win = banded_attention(q_a, heads(a_kw, A_KV_HEADS), heads(a_vw, A_KV_HEADS), slopes_a, NSA_WINDOW, None)
    gate = jax.nn.sigmoid(heads(a_gate, A_HEADS))
    o_a = gate[..., 0:1] * o_cmp + gate[..., 1:2] * o_slc + gate[..., 2:3] * o_win
    y_a = o_a.reshape(bsz, T, A_WIDTH) * jax.nn.silu(a_z)

    qk_b = jax.nn.silu(causal_depthwise_conv(b_qk, conv_w, conv_b))
    q_b, k_b = jnp.split(qk_b, 2, axis=-1)
    h_b = mlstm_chunkwise(heads(q_b, B_HEADS).astype(f32),
                          heads(k_b, B_HEADS).astype(f32) * B_HEAD_DIM ** -0.5,
                          heads(b_v, B_HEADS).astype(f32),
                          (b_i + i_bias).astype(f32),
                          (b_f + f_bias).astype(f32))
    h_b = jax.nn.sigmoid(heads(b_o, B_HEADS).astype(f32)) * h_b
    h_b = rmsnorm(h_b, mnorm_g.reshape(B_HEADS, B_HEAD_DIM)).astype(x.dtype)
    y_b = h_b.reshape(bsz, T, B_WIDTH) * jax.nn.silu(b_z)

    o_c = banded_attention(heads(c_q, C_HEADS), heads(c_k, C_KV_HEADS), heads(c_v, C_KV_HEADS),
                           alibi_slopes(C_HEADS), SWA_WINDOW, sinks)
    y_c = o_c.reshape(bsz, T, C_WIDTH) * jax.nn.silu(c_z)

    mix = jnp.concatenate([y_a, y_b, y_c], axis=-1)
    return x + mix @ w_out


def setup_inputs(seed: int = 0) -> dict:
    key = jax.random.key(seed)
    ks = jax.random.split(key, 17)
    f32 = jnp.float32

    def nrm(k, shape, s):
        return jax.random.normal(k, shape, f32) * s

    cmp_in = CMP_BLOCK * HEAD_DIM
    return {
        'x': nrm(ks[0], (BATCH, SEQ, D_MODEL), 1.0),
        'norm_g': 1.0 + nrm(ks[1], (DEPTH, D_MODEL), 0.02),
        'w_in': nrm(ks[2], (DEPTH, D_MODEL, IN_COLS), D_MODEL ** -0.5),
        'w_out': nrm(ks[3], (DEPTH, MIX_WIDTH, D_MODEL), MIX_WIDTH ** -0.5),
        'cmp_pe_k': nrm(ks[4], (DEPTH, CMP_BLOCK, HEAD_DIM), 0.02),
        'cmp_w1_k': nrm(ks[5], (DEPTH, cmp_in, HEAD_DIM), cmp_in ** -0.5),
        'cmp_w2_k': nrm(ks[6], (DEPTH, HEAD_DIM, HEAD_DIM), HEAD_DIM ** -0.5),
        'cmp_pe_v': nrm(ks[7], (DEPTH, CMP_BLOCK, HEAD_DIM), 0.02),
        'cmp_w1_v': nrm(ks[8], (DEPTH, cmp_in, HEAD_DIM), cmp_in ** -0.5),
        'cmp_w2_v': nrm(ks[9], (DEPTH, HEAD_DIM, HEAD_DIM), HEAD_DIM ** -0.5),
        'mlstm_conv_w': nrm(ks[10], (DEPTH, CONV_WIDTH, 2 * B_WIDTH), CONV_WIDTH ** -0.5),
        'mlstm_conv_b': nrm(ks[11], (DEPTH, 2 * B_WIDTH), 0.01),
        'mlstm_i_bias': nrm(ks[12], (DEPTH, B_HEADS), 0.1),
        'mlstm_f_bias': jnp.linspace(3.0, 6.0, B_HEADS, dtype=f32)[None, :] + nrm(ks[13], (DEPTH, B_HEADS), 0.1),
        'mlstm_norm_g': 1.0 + nrm(ks[14], (DEPTH, B_WIDTH), 0.02),
        'swa_sinks': nrm(ks[15], (DEPTH, C_HEADS), 0.5),
        'final_norm_g': 1.0 + nrm(ks[16], (D_MODEL,), 0.02),
    }


def reference(x, norm_g, w_in, w_out, cmp_pe_k, cmp_w1_k, cmp_w2_k, cmp_pe_v, cmp_w1_v, cmp_w2_v,
              mlstm_conv_w, mlstm_conv_b, mlstm_i_bias, mlstm_f_bias, mlstm_norm_g, swa_sinks, final_norm_g):
    for l in range(DEPTH):
        x = hybrid_layer(x, norm_g[l], w_in[l], w_out[l],
                         cmp_pe_k[l], cmp_w1_k[l], cmp_w2_k[l], cmp_pe_v[l], cmp_w1_v[l], cmp_w2_v[l],
                         mlstm_conv_w[l], mlstm_conv_b[l], mlstm_i_bias[l], mlstm_f_bias[l],
                         mlstm_norm_g[l], swa_sinks[l])
    return rmsnorm(x, final_norm_g)
```

```python
import contextlib
import numpy as np
import ml_dtypes
import concourse.bass as bass
import concourse.mybir as mybir
from concourse.bass_utils import run_bass_kernel_spmd

F32 = mybir.dt.float32
BF16 = mybir.dt.bfloat16
AF = mybir.ActivationFunctionType
ALU = mybir.AluOpType
AX = mybir.AxisListType
NPBF = ml_dtypes.bfloat16

T = 8192
D = 2048
TQ = 2048
NEG = -30000.0
EPS = 1.0e-6
NF = 1280
NT = 1222


class Sched:
    COMPUTE = ("pe", "act", "dve", "pool")

    def __init__(self, nc, stack, n_slots=24, dma_queues=("sp", "pool")):
        self.nc = nc
        self.eng = {"pe": nc.tensor, "act": nc.scalar, "dve": nc.vector,
                    "pool": nc.gpsimd, "sp": nc.sync}
        self.ops = []
        self.lastw = {}
        self.readers = {}
        self.n_slots = n_slots
        self.slot_last = [None] * n_slots
        self.slot_next = 0
        self.sem = {e: stack.enter_context(nc.semaphore("s_" + e)) for e in self.COMPUTE}
        self.slot_sem = [stack.enter_context(nc.semaphore("d_%d" % i)) for i in range(n_slots)]
        self.dma_queues = dma_queues
        self.dma_rr = 0
        self.cnt = {e: 0 for e in self.COMPUTE}
        self.slot_cnt = [0] * n_slots
        self.waited = {}

    def _deps(self, reads, writes):
        deps = set()
        for k in list(reads) + list(writes):
            w = self.lastw.get(k)
            if w is not None:
                deps.add(w)
        for k in writes:
            r = self.readers.get(k)
            if r:
                for e, v in r.items():
                    if e == "_dma":
                        deps.update(v)
                    else:
                        deps.add(v)
        return deps

    def _update(self, oid, eng, is_dma, reads, writes):
        for k in writes:
            self.lastw[k] = oid
            self.readers[k] = {}
        ws = set(writes)
        for k in reads:
            if k in ws:
                continue
            r = self.readers.setdefault(k, {})
            if is_dma:
                r.setdefault("_dma", []).append(oid)
            else:
                r[eng] = oid

    @staticmethod
    def _excl(reads, writes):
        ex = [k for k in reads if (isinstance(k, tuple) and str(k[0]).startswith("ps")) or
              (isinstance(k, str) and k.startswith("ps"))]
        if ex:
            writes = list(writes) + [k for k in ex if k not in writes]
        return reads, writes

    def op(self, eng, fn, reads=(), writes=()):
        reads, writes = self._excl(reads, writes)
        deps = self._deps(reads, writes)
        oid = len(self.ops)
        self.ops.append(dict(eng=eng, fn=fn, deps=deps, dma=False, slot=None))
        self._update(oid, eng, False, reads, writes)
        return oid

    def dma(self, fn, reads=(), writes=(), queue=None):
        if queue is None:
            queue = self.dma_queues[self.dma_rr % len(self.dma_queues)]
            self.dma_rr += 1
        deps = self._deps(reads, writes)
        slot = self.slot_next
        self.slot_next = (self.slot_next + 1) % self.n_slots
        if self.slot_last[slot] is not None:
            deps.add(self.slot_last[slot])
        oid = len(self.ops)
        self.slot_last[slot] = oid
        self.ops.append(dict(eng=queue, fn=fn, deps=deps, dma=True, slot=slot))
        self._update(oid, queue, True, reads, writes)
        return oid

    def emit(self):
        ops = self.ops
        n = len(ops)
        signal = [False] * n
        for i, o in enumerate(ops):
            if o is None:
                continue
            if o["dma"]:
                signal[i] = True
            for d in o["deps"]:
                od = ops[d]
                if od is None or od["dma"]:
                    continue
                if od["eng"] == o["eng"] and not o["dma"] and o["eng"] == "pe":
                    continue
                signal[d] = True
        val = [None] * n
        for i, o in enumerate(ops):
            if o is None:
                continue
            if o["dma"]:
                self.slot_cnt[o["slot"]] += 16
                val[i] = (self.slot_sem[o["slot"]], self.slot_cnt[o["slot"]], ("slot", o["slot"]))
            elif signal[i]:
                self.cnt[o["eng"]] += 1
                val[i] = (self.sem[o["eng"]], self.cnt[o["eng"]], ("eng", o["eng"]))
        nwait = 0
        for i, o in enumerate(ops):
            if o is None:
                continue
            e = self.eng[o["eng"]]
            need = {}
            for d in o["deps"]:
                od = ops[d]
                if od is None:
                    continue
                if (not od["dma"]) and od["eng"] == o["eng"] and o["eng"] == "pe" and not o["dma"]:
                    continue
                s, v, key = val[d]
                if need.get(key, (None, 0))[1] < v:
                    need[key] = (s, v)
            for key, (s, v) in need.items():
                wk = (o["eng"], key)
                if self.waited.get(wk, 0) >= v:
                    continue
                e.wait_ge(s, v)
                self.waited[wk] = v
                nwait += 1
            inst = o["fn"](e)
            if o["dma"]:
                inst.then_inc(val[i][0], 16)
            elif signal[i]:
                inst.then_inc(val[i][0], 1)
        for en in ("sp", "pe", "act", "dve", "pool"):
            e = self.eng[en]
            for s in range(self.n_slots):
                if self.slot_cnt[s] and self.waited.get((en, ("slot", s)), 0) < self.slot_cnt[s]:
                    e.wait_ge(self.slot_sem[s], self.slot_cnt[s])
                    self.waited[(en, ("slot", s))] = self.slot_cnt[s]
            for ce in self.COMPUTE:
                if ce == en or not self.cnt[ce]:
                    continue
                if self.waited.get((en, ("eng", ce)), 0) < self.cnt[ce]:
                    e.wait_ge(self.sem[ce], self.cnt[ce])
                    self.waited[(en, ("eng", ce))] = self.cnt[ce]
        stats = dict(n_ops=n, n_wait=nwait, n_signal=sum(signal))
        self.ops = []
        self.lastw = {}
        self.readers = {}
        self.slot_last = [None] * self.n_slots
        return stats


class Ctx:
    def __init__(self, nc, st, io):
        self.nc = nc
        self.st = st
        self.io = io
        self.S = Sched(nc, st)
        self.dr = {}
        self.rr = 0

    def dram(self, name, shape, dt):
        if name in self.dr:
            return self.dr[name]
        kind = self.io.get(name)
        if kind == "in":
            t = self.nc.dram_tensor(name, list(shape), dt, kind="ExternalInput")
        elif kind == "out":
            t = self.nc.dram_tensor(name, list(shape), dt, kind="ExternalOutput")
        else:
            t = self.nc.dram_tensor(name, list(shape), dt)
        self.dr[name] = t.ap()
        return self.dr[name]

    def end_phase(self):
        return self.S.emit()

    def copy(self, eng, out, in_, reads, writes):
        if eng == "act":
            self.S.op("act", lambda e: e.activation(out=out, in_=in_, func=AF.Copy), reads, writes)
        else:
            self.S.op(eng, lambda e: e.tensor_copy(out=out, in_=in_), reads, writes)

    def alt(self):
        self.rr += 1
        return "act" if self.rr % 2 else "dve"

    def load(self, out, in_, reads, writes, queue=None):
        self.S.dma(lambda e: e.dma_start(out=out, in_=in_), reads, writes, queue)

    def mm(self, out, lhsT, rhs, start, stop, reads, writes):
        self.S.op("pe", lambda e: e.matmul(out, lhsT=lhsT, rhs=rhs, start=start, stop=stop,
                                           skip_group_check=True), reads, writes)


def phase_ON(C, tag, do_O, last, xin_name, xout_name, h_name):
    nc, S = C.nc, C.S
    xin = C.dram(xin_name, [D, TQ], F32)
    g = C.dram("g_" + tag, [128, 16], F32)
    ones_d = C.dram("ones_f32", [128, 128], F32)
    if do_O:
        mixo = C.dram("mixo_" + tag, [4, 512, TQ], BF16)
        wout = C.dram("wout_" + tag, [D, D], F32)
    if last:
        outT = C.dram("outT", [D, TQ], F32)
    else:
        xout = C.dram(xout_name, [D, TQ], F32) if do_O else None
        hT = C.dram(h_name, [D, TQ], BF16)
    with contextlib.ExitStack() as st:
        def sb(name, shape, dt):
            return st.enter_context(nc.sbuf_tensor("sb_" + name + "_" + tag, shape, dt))
        xt = [sb("xt%d" % i, [128, 16, 512], F32) for i in range(2 if do_O else 1)]
        gt = sb("gt", [128, 16], F32)
        ones = sb("ones", [128, 128], F32)
        sqc = [sb("sqc%d" % i, [128, 512], F32) for i in range(2)]
        sd = sb("sd", [128, 512], F32)
        rstd = sb("rstd", [128, 512], F32)
        hb = sb("hb", [128, 16, 512], BF16) if not last else None
        ps_ss = st.enter_context(nc.psum_tensor("ps_ss_" + tag, [128, 512], F32))
        C.load(gt[:], g, [], ["gt"])
        C.load(ones[:], ones_d, [], ["ones"])
        if do_O:
            wo = sb("wo", [128, 16, D], BF16)
            wst = [sb("wst%d" % i, [128, D], F32) for i in range(2)]
            mt = [sb("mt%d" % i, [128, 16, 512], BF16) for i in range(2)]
            pso = [st.enter_context(nc.psum_tensor("pso%d_%s" % (i, tag), [128, 512], F32)) for i in range(4)]
            for kc in range(16):
                w = wst[kc % 2]
                C.load(w[:], wout[kc * 128:(kc + 1) * 128, :], [], [("wst", kc % 2)])
                C.copy(C.alt(), wo[:, kc, :], w[:], [("wst", kc % 2)], [("wo", kc)])
        xin_v = xin.rearrange("(c p) t -> p c t", p=128)
        for tt in range(4):
            tsl = slice(tt * 512, (tt + 1) * 512)
            x = xt[tt % len(xt)]
            xk = ("xt", tt % len(xt))
            for hh in range(2):
                C.load(x[:, hh * 8:(hh + 1) * 8, :], xin_v[:, hh * 8:(hh + 1) * 8, tsl], [], [xk])
            if do_O:
                m = mt[tt % 2]
                mk = ("mt", tt % 2)
                mv = mixo.rearrange("r (c p) t -> p r c t", p=128)
                for r in range(4):
                    C.load(m[:, r * 4:(r + 1) * 4, :], mv[:, r, :, tsl], [], [mk])
                for fc in range(16):
                    ps = pso[fc % 4]
                    pk = ("pso", fc % 4)
                    for kc in range(16):
                        C.mm(ps[:], wo[:, kc, fc * 128:(fc + 1) * 128], m[:, kc, :], kc == 0, kc == 15,
                             [("wo", kc), mk], [pk])
                    S.op("dve", lambda e, ps=ps, x=x, fc=fc: e.tensor_tensor(
                        out=x[:, fc, :], in0=ps[:], in1=x[:, fc, :], op=ALU.add), [pk, xk], [xk])
            for c in range(16):
                sq = sqc[c % 2]
                S.op("act", lambda e, sq=sq, x=x, c=c: e.activation(out=sq[:], in_=x[:, c, :], func=AF.Square),
                     [xk], [("sqc", c % 2)])
                C.mm(ps_ss[:], ones[:], sq[:], c == 0, c == 15, ["ones", ("sqc", c % 2)], ["ps_ss"])
            S.op("act", lambda e: e.activation(out=sd[:], in_=ps_ss[:], func=AF.Sqrt, bias=EPS, scale=1.0 / D),
                 ["ps_ss"], ["sd"])
            S.op("dve", lambda e: e.reciprocal(out=rstd[:], in_=sd[:]), ["sd"], ["rstd"])
            if not last:
                for hh in range(2 if do_O else 0):
                    C.load(xout.rearrange("(c p) t -> p c t", p=128)[:, hh * 8:(hh + 1) * 8, tsl],
                           x[:, hh * 8:(hh + 1) * 8, :], [xk], [("xout", tt)])
                for c in range(16):
                    S.op("dve", lambda e, x=x, c=c: e.scalar_tensor_tensor(
                        out=hb[:, c, :], in0=x[:, c, :], scalar=gt[:, c:c + 1], in1=rstd[:],
                        op0=ALU.mult, op1=ALU.mult), [xk, "gt", "rstd"], ["hb"])
                for hh in range(2):
                    C.load(hT.rearrange("(c p) t -> p c t", p=128)[:, hh * 8:(hh + 1) * 8, tsl],
                           hb[:, hh * 8:(hh + 1) * 8, :], ["hb"], [("hT", tt)])
            else:
                for c in range(16):
                    S.op("dve", lambda e, x=x, c=c: e.scalar_tensor_tensor(
                        out=x[:, c, :], in0=x[:, c, :], scalar=gt[:, c:c + 1], in1=rstd[:],
                        op0=ALU.mult, op1=ALU.mult), [xk, "gt", "rstd"], [xk])
                for hh in range(2):
                    C.load(outT.rearrange("(c p) t -> p c t", p=128)[:, hh * 8:(hh + 1) * 8, tsl],
                           x[:, hh * 8:(hh + 1) * 8, :], [xk], [("outT", tt)])
        return C.end_phase()


def scratch_P(C, tag):
    d = {}
    d["qA"] = C.dram("qA_" + tag, [4, 64, T], BF16)
    d["kcvc"] = C.dram("kcvc_" + tag, [2, 64, T + 32], BF16)
    d["kskw"] = C.dram("kskw_" + tag, [2, 64, T], BF16)
    d["qC"] = C.dram("qC_" + tag, [2, 64, T], BF16)
    d["kC"] = C.dram("kC_" + tag, [64, T], BF16)
    d["gif"] = C.dram("gif_" + tag, [2, T], F32)
    d["bqk"] = C.dram("bqk_" + tag, [4, 128, T], BF16)
    d["vst"] = C.dram("vst_" + tag, [128, 3, 64, 65], BF16)
    d["gst"] = C.dram("gst_" + tag, [128, 64, 6], F32)
    d["zAC"] = C.dram("zAC_" + tag, [128, 64, 256], BF16)
    d["t2"] = C.dram("t2_" + tag, [T, 512], BF16)
    d["t3"] = C.dram("t3_" + tag, [T, 256], BF16)
    return d


def phase_P(C, tag, n_tiles=16, dbg=()):
    nc, S = C.nc, C.S
    hg = C.dram("hg_" + tag, [4, D, TQ], BF16)
    wpm = C.dram("wpm_" + tag, [D, NF + NT], F32)
    cwb_d = C.dram("cwb_" + tag, [128, 4, 5], F32)
    sc = scratch_P(C, tag)
    with contextlib.ExitStack() as st:
        def sb(name, shape, dt):
            return st.enter_context(nc.sbuf_tensor("sb_" + name + "_" + tag, shape, dt))
        W = sb("W", [128, 16, NF + NT], BF16)
        wst = [sb("wst%d" % i, [128, NF + NT], F32) for i in range(2)]
        ht = [sb("ht%d" % i, [128, 16, 512], BF16) for i in range(2)]
        cwb = sb("cwb", [128, 4, 5], F32)
        cst = sb("cst", [128, 4, 515], F32)
        acc = [sb("acc%d" % i, [128, 512], F32) for i in range(2)]
        sg = [sb("sg%d" % i, [128, 512], F32) for i in range(2)]
        ob = [sb("ob%d" % i, [128, 512], BF16) for i in range(4)]
        g2 = sb("g2", [128, 512], F32)
        vst = sb("vst", [128, 3, 64, 65], BF16)
        gst = sb("gst", [128, 64, 6], F32)
        zst = [sb("zst%d" % i, [128, 256], BF16) for i in range(2)]
        t2s = [sb("t2s%d" % i, [128, 512], BF16) for i in range(2)]
        t3s = [sb("t3s%d" % i, [128, 256], BF16) for i in range(2)]
        zpad = sb("zpad", [128, 32], BF16)
        psf = [st.enter_context(nc.psum_tensor("psf%d_%s" % (i, tag), [128, 512], F32)) for i in range(4)]
        pst = [st.enter_context(nc.psum_tensor("pst%d_%s" % (i, tag), [128, 512], F32)) for i in range(4)]
        C.load(cwb[:], cwb_d, [], ["cwb"])
        S.op("pool", lambda e: e.memset(cst[:], 0.0), [], [("cst", i) for i in range(4)])
        S.op("pool", lambda e: e.memset(vst[:], 1.0), [], ["vst"])
        S.op("pool", lambda e: e.memset(zpad[:], 0.0), [], ["zpad"])
        C.load(sc["kcvc"].rearrange("a d t -> (a d) t")[:, T:T + 32], zpad[:], ["zpad"], [("kcvc", "pad")])
        for kc in range(16):
            w = wst[kc % 2]
            C.load(w[:], wpm[kc * 128:(kc + 1) * 128, :], [], [("wst", kc % 2)])
            C.copy(C.alt(), W[:, kc, :], w[:], [("wst", kc % 2)], [("W", kc)])
        Wk = [("W", kc) for kc in range(16)]
        obi = 0
        psti = 0
        for tt in range(n_tiles):
            r, lt = tt // 4, tt % 4
            h = ht[tt % 2]
            hk = ("ht", tt % 2)
            hv = hg[r].rearrange("(c p) t -> p c t", p=128)
            for hh in range(2):
                C.load(h[:, hh * 8:(hh + 1) * 8, :], hv[:, hh * 8:(hh + 1) * 8, lt * 512:(lt + 1) * 512], [], [hk])
            tsl = slice(tt * 512, (tt + 1) * 512)
            for c in range(10):
                if ('nofm' in dbg) or ('noconv' in dbg and c >= 6):
                    continue
                if any(d.startswith('c=') for d in dbg) and str(c) not in [d for d in dbg if d.startswith('c=')][0][2:].split('+'):
                    continue
                ps = psf[c % 4]
                pk = ("psf", c % 4)
                for kc in range(16):
                    C.mm(ps[:], W[:, kc, c * 128:(c + 1) * 128], h[:, kc, :], kc == 0, kc == 15, [("W", kc), hk], [pk])
                if c < 6:
                    o = ob[obi % 4]
                    ok = ("ob", obi % 4)
                    obi += 1
                    C.copy('act' if (c == 5 or 'allact' in dbg) else C.alt(), o[:], ps[:], [pk], [ok])
                    if c == 0:
                        C.load(sc["qA"][0:2].rearrange("a d t -> (a d) t")[:, tsl], o[:], [ok], [("qA", tt)])
                    elif c == 1:
                        C.load(sc["qA"][2:4].rearrange("a d t -> (a d) t")[:, tsl], o[:], [ok], [("qA2", tt)])
                    elif c == 2:
                        C.load(sc["kcvc"].rearrange("a d t -> (a d) t")[:, tsl], o[:], [ok], [("kcvc", tt)])
                    elif c == 3:
                        C.load(sc["kskw"].rearrange("a d t -> (a d) t")[:, tsl], o[:], [ok], [("kskw", tt)])
                    elif c == 4:
                        C.load(sc["qC"].rearrange("a d t -> (a d) t")[:, tsl], o[:], [ok], [("qC", tt)])
                    else:
                        C.load(sc["kC"][:, tsl], o[0:64, :], [ok], [("kC", tt)])
                        S.op("act", lambda e, ps=ps: e.activation(out=g2[64:66, :], in_=ps[64:66, :], func=AF.Copy),
                             [pk], ["g2"])
                        C.load(sc["gif"][:, tsl], g2[64:66, :], ["g2"], [("gif", tt)])
                else:
                    ci = c - 6
                    ck = ("cst", ci)
                    S.op("act", lambda e, ps=ps, ci=ci: e.activation(out=cst[:, ci, 3:515], in_=ps[:], func=AF.Copy),
                         [pk], [ck])
                    a = acc[ci % 2]
                    ak = ("acc", ci % 2)
                    S.op("dve", lambda e, a=a, ci=ci: e.tensor_scalar(
                        out=a[:], in0=cst[:, ci, 3:515], scalar1=cwb[:, ci, 3:4], scalar2=cwb[:, ci, 4:5],
                        op0=ALU.mult, op1=ALU.add), [ck, "cwb"], [ak])
                    for tap in (2, 1, 0):
                        S.op("dve", lambda e, a=a, ci=ci, tap=tap: e.scalar_tensor_tensor(
                            out=a[:], in0=cst[:, ci, tap:tap + 512], scalar=cwb[:, ci, tap:tap + 1], in1=a[:],
                            op0=ALU.mult, op1=ALU.add), [ck, "cwb", ak], [ak])
                    S.op("pool", lambda e, ci=ci: e.tensor_copy(out=cst[:, ci, 0:3], in_=cst[:, ci, 512:515]), [ck], [ck])
                    s_ = sg[ci % 2]
                    sk = ("sg", ci % 2)
                    S.op("act", lambda e, a=a, s_=s_: e.activation(out=s_[:], in_=a[:], func=AF.Sigmoid), [ak], [sk])
                    o = ob[obi % 4]
                    ok = ("ob", obi % 4)
                    obi += 1
                    S.op("dve", lambda e, a=a, s_=s_, o=o: e.tensor_tensor(out=o[:], in0=a[:], in1=s_[:], op=ALU.mult),
                         [ak, sk], [ok])
                    C.load(sc["bqk"][ci][:, tsl], o[:], [ok], [("bqk", ci, tt)])
            for sub in range(4):
                if 'notm' in dbg:
                    continue
                n = tt * 4 + sub
                hs = slice(sub * 128, (sub + 1) * 128)
                ps = pst[psti % 4]
                pk = ("pst", psti % 4)
                psti += 1
                for kc in range(16):
                    C.mm(ps[:, 0:454], h[:, kc, hs], W[:, kc, NF:NF + 454], kc == 0, kc == 15, [("W", kc), hk], [pk])
                s_ = sg[sub % 2]
                sk = ("sg", sub % 2)
                S.op("act", lambda e, ps=ps, s_=s_: e.activation(out=s_[:, 0:262], in_=ps[:, 192:454], func=AF.Sigmoid),
                     [pk], [sk])
                S.op("dve", lambda e, ps=ps, n=n: e.tensor_copy(
                    out=vst[:, :, n, 0:64], in_=ps[:, 0:192].rearrange("p (a c) -> p a c", a=3)), [pk], ["vst"])
                z = zst[sub % 2]
                zk = ("zst", sub % 2)
                S.op("dve", lambda e, ps=ps, s_=s_, z=z: e.tensor_tensor(
                    out=z[:], in0=ps[:, 198:454], in1=s_[:, 6:262], op=ALU.mult), [pk, sk], [zk])
                S.op("pool", lambda e, s_=s_, n=n: e.tensor_copy(out=gst[:, n, :], in_=s_[:, 0:6]), [sk], ["gst"])
                C.load(sc["zAC"][:, n, :], z[:], [zk], [("zAC", n)])
                ps = pst[psti % 4]
                pk = ("pst", psti % 4)
                psti += 1
                for kc in range(16):
                    C.mm(ps[:], h[:, kc, hs], W[:, kc, NF + 454:NF + 966], kc == 0, kc == 15, [("W", kc), hk], [pk])
                t2 = t2s[sub % 2]
                tk = ("t2s", sub % 2)
                S.op("dve", lambda e, ps=ps, t2=t2: e.tensor_copy(out=t2[:, 0:256], in_=ps[:, 0:256]), [pk], [tk])
                S.op("act", lambda e, ps=ps, t2=t2: e.activation(out=t2[:, 256:512], in_=ps[:, 256:512], func=AF.Sigmoid),
                     [pk], [tk])
                C.load(sc["t2"][n * 128:(n + 1) * 128, :], t2[:], [tk], [("t2", n)])
                ps = pst[psti % 4]
                pk = ("pst", psti % 4)
                psti += 1
                for kc in range(16):
                    C.mm(ps[:, 0:256], h[:, kc, hs], W[:, kc, NF + 966:NF + 1222], kc == 0, kc == 15, [("W", kc), hk], [pk])
                s3 = acc[sub % 2]
                s3k = ("acc", sub % 2)
                S.op("act", lambda e, ps=ps, s3=s3: e.activation(out=s3[:, 0:256], in_=ps[:, 0:256], func=AF.Sigmoid),
                     [pk], [s3k])
                t3 = t3s[sub % 2]
                t3k = ("t3s", sub % 2)
                S.op("dve", lambda e, ps=ps, s3=s3, t3=t3: e.tensor_tensor(
                    out=t3[:], in0=ps[:, 0:256], in1=s3[:, 0:256], op=ALU.mult), [pk, s3k], [t3k])
                C.load(sc["t3"][n * 128:(n + 1) * 128, :], t3[:], [t3k], [("t3", n)])
        C.load(sc["vst"], vst[:], ["vst"], [("vstd",)])
        C.load(sc["gst"], gst[:], ["gst"], [("gstd",)])
        return C.end_phase()


OFF = dict(a_q=0, a_kc=512, a_vc=640, a_ks=768, a_vs=896, a_kw=1024, a_vw=1152, a_gate=1280, a_z=1304,
           b_qk=1816, b_v=3864, b_i=4888, b_f=4892, b_o=4896, b_z=5920, c_q=6944, c_k=7456, c_v=7584, c_z=7712)


def _rng(a, n):
    return list(range(a, a + n))


def wpm_cols(hq):
    g = hq // 2
    own = [2 * hq, 2 * hq + 1]
    oth = [h for h in range(4 * g, 4 * g + 4) if h not in own]
    c = []
    for h in own + oth:
        c += _rng(OFF["a_q"] + 64 * h, 64)
    c += _rng(OFF["a_kc"] + 64 * g, 64) + _rng(OFF["a_vc"] + 64 * g, 64)
    c += _rng(OFF["a_ks"] + 64 * g, 64) + _rng(OFF["a_kw"] + 64 * g, 64)
    for h in own:
        c += _rng(OFF["c_q"] + 64 * h, 64)
    c += _rng(OFF["c_k"] + 64 * g, 64) + [OFF["b_i"] + hq, OFF["b_f"] + hq] + [-1] * 62
    c += _rng(OFF["b_qk"] + 256 * hq, 256)
    c += _rng(OFF["b_qk"] + 1024 + 256 * hq, 256)
    assert len(c) == NF
    c += _rng(OFF["a_vs"] + 64 * g, 64) + _rng(OFF["a_vw"] + 64 * g, 64) + _rng(OFF["c_v"] + 64 * g, 64)
    for h in own:
        c += _rng(OFF["a_gate"] + 3 * h, 3)
    for h in own:
        c += _rng(OFF["a_z"] + 64 * h, 64)
    for h in own:
        c += _rng(OFF["c_z"] + 64 * h, 64)
    c += _rng(OFF["b_v"] + 256 * hq, 256) + _rng(OFF["b_o"] + 256 * hq, 256)
    c += _rng(OFF["b_z"] + 256 * hq, 256)
    assert len(c) == NF + NT
    return np.array(c)


def take_cols(w, cols):
    out = np.zeros((w.shape[0], len(cols)), w.dtype)
    m = cols >= 0
    out[:, m] = w[:, cols[m]]
    return out


def prep_cwb(conv_w, conv_b, hq):
    out = np.zeros((128, 4, 5), np.float32)
    for ci in range(4):
        base = (256 * hq + 128 * ci) if ci < 2 else (1024 + 256 * hq + 128 * (ci - 2))
        out[:, ci, 0:4] = conv_w[:, base:base + 128].T
        out[:, ci, 4] = conv_b[base:base + 128]
    return out


def const_tables():
    kl = np.arange(128)[:, None]
    ql = np.arange(512)[None, :]
    t = {}
    t["ident_bf"] = np.eye(128, dtype=np.float32).astype(NPBF)
    band = np.zeros((128, 5, 512), np.float32)
    for r in range(5):
        d = ql - kl - 128 * (r - 1)
        band[:, r, :] = np.where((d >= 0) & (d < 128), 0.0, NEG)
    t["band"] = band.astype(NPBF)
    tri = np.zeros((128, 4, 512), np.float32)
    low = np.zeros((128, 4, 512), np.float32)
    for r in range(4):
        tri[:, r, :] = np.where(128 * r + kl <= ql, 0.0, NEG)
        low[:, r, :] = np.where(kl + 128 * r > ql, 0.0, NEG)
    t["tri"] = tri.astype(NPBF)
    t["low"] = low.astype(NPBF)
    vis = np.zeros((128, 5, 512), np.float32)
    for m in range(5):
        vis[:, m, :] = np.where(16 * kl + 31 - ql <= 512 * m, 0.0, NEG)
    t["vis"] = vis.astype(NPBF)
    pos = np.arange(T)
    kaug = np.stack([np.ones(T), np.ones(T), pos // 64, pos % 64]).astype(np.float32)
    t["kaug"] = kaug.astype(NPBF)
    return t


def qaug_table(hq):
    pos = np.arange(T)
    out = np.zeros((2, 4, T), np.float32)
    for i, h in enumerate((2 * hq, 2 * hq + 1)):
        sl = 2.0 ** (-(h + 1))
        out[i, 0] = -8.0 * sl * 64.0 * (pos // 64)
        out[i, 1] = -8.0 * sl * (pos % 64)
        out[i, 2] = 8.0 * sl * 64.0
        out[i, 3] = 8.0 * sl
    return out.astype(NPBF)


def phase_MC(C, tag, n_qt=16):
    nc, S = C.nc, C.S
    sc = scratch_P(C, tag)
    qaug_d = C.dram("qaug", [2, 4, T], BF16)
    kaug_d = C.dram("kaug", [4, T], BF16)
    band_d = C.dram("band", [128, 5, 512], BF16)
    ident_d = C.dram("ident_bf", [128, 128], BF16)
    sink_d = C.dram("sinks_" + tag, [128, 2], F32)
    yT = C.dram("yT_" + tag, [512, T], BF16)
    with contextlib.ExitStack() as st:
        def sb(name, shape, dt):
            return st.enter_context(nc.sbuf_tensor("sb_" + name + "_mc" + tag, shape, dt))
        ka = sb("ka", [68, T], BF16)
        va = sb("va", [128, 64, 65], BF16)
        band = sb("band", [128, 5, 512], BF16)
        ident = sb("ident", [128, 128], BF16)
        esink = sb("esink", [128, 2], F32)
        qa = [[sb("qa%d_%d" % (i, h), [68, 512], BF16) for h in range(2)] for i in range(2)]
        zt = [sb("zt%d" % i, [128, 4, 128], BF16) for i in range(2)]
        PT = [sb("PT%d" % i, [128, 512], BF16) for i in range(3)]
        den = sb("den", [128, 4], F32)
        rden = sb("rden", [128, 4], F32)
        tmp = sb("tmp", [128, 4, 64], F32)
        ybf = sb("ybf", [128, 4, 128], BF16)
        ysb = [sb("ysb%d" % i, [128, 512], BF16) for i in range(2)]
        pss = [st.enter_context(nc.psum_tensor("pss%d_mc%s" % (i, tag), [128, 512], F32)) for i in range(3)]
        pso = [st.enter_context(nc.psum_tensor("pso%d_mc%s" % (i, tag), [128, 4, 65], F32)) for i in range(2)]
        pst = st.enter_context(nc.psum_tensor("pst_mc%s" % tag, [128, 4, 128], BF16))
        C.load(ka[0:64, :], sc["kC"], [("kCd",)], ["ka"])
        C.load(ka[64:68, :], kaug_d, [], ["ka"])
        C.load(va[:], sc["vst"][:, 2], [("vstd",)], ["va"])
        C.load(band[:], band_d, [], ["band"])
        C.load(ident[:], ident_d, [], ["ident"])
        C.load(esink[:], sink_d, [], ["esink"])
        S.op("act", lambda e: e.activation(out=esink[:], in_=esink[:], func=AF.Exp), ["esink"], ["esink"])
        si = 0
        for qt in range(n_qt):
            qsl = slice(qt * 512, (qt + 1) * 512)
            z = zt[qt % 2]
            zk = ("zt", qt % 2)
            C.load(z[:], sc["zAC"][:, 4 * qt:4 * qt + 4, 128:256], [("zACd",)], [zk])
            for h in range(2):
                q = qa[qt % 2][h]
                qk = ("qa", qt % 2, h)
                C.load(q[0:64, :], sc["qC"][h][:, qsl], [("qCd",)], [qk])
                C.load(q[64:68, :], qaug_d[h][:, qsl], [], [qk])
            for h in range(2):
                q = qa[qt % 2][h]
                qk = ("qa", qt % 2, h)
                po = pso[h]
                pok = ("pso", h)
                kts = [(4 * qt - 1 + r, r) for r in range(5) if 4 * qt - 1 + r >= 0]
                for idx, (kt, r) in enumerate(kts):
                    ps = pss[si % 3]
                    pk = ("pss", si % 3)
                    pt = PT[si % 3]
                    ptk = ("PT", si % 3)
                    si += 1
                    C.mm(ps[:], ka[0:68, kt * 128:(kt + 1) * 128], q[0:68, :], True, False, ["ka", qk], [pk])
                    C.mm(ps[:], ident[:], band[:, r, :], False, True, ["ident", "band"], [pk])
                    S.op("act", lambda e, ps=ps, pt=pt: e.activation(out=pt[:], in_=ps[:], func=AF.Exp, scale=0.125),
                         [pk], [ptk])
                    for sub in range(4):
                        C.mm(po[:, sub, :], pt[:, sub * 128:(sub + 1) * 128], va[:, kt, :],
                             idx == 0 and sub == 0, idx == len(kts) - 1, [ptk, "va"], [pok])
                S.op("dve", lambda e, po=po, h=h: e.tensor_scalar(
                    out=den[:], in0=po[:, :, 64], scalar1=esink[:, h:h + 1], scalar2=None, op0=ALU.add),
                    [pok, "esink"], ["den"])
                S.op("dve", lambda e: e.reciprocal(out=rden[:], in_=den[:]), ["den"], ["rden"])
                S.op("dve", lambda e, po=po: e.tensor_tensor(
                    out=tmp[:], in0=po[:, :, 0:64], in1=rden[:].unsqueeze(2).broadcast_to([128, 4, 64]), op=ALU.mult),
                    [pok, "rden"], ["tmp"])
                S.op("pool", lambda e, z=z, h=h: e.tensor_tensor(
                    out=ybf[:, :, h * 64:(h + 1) * 64], in0=tmp[:], in1=z[:, :, h * 64:(h + 1) * 64], op=ALU.mult),
                    ["tmp", zk], ["ybf"])
            for sub in range(4):
                S.op("pe", lambda e, sub=sub: e.transpose(out=pst[:, sub, :], in_=ybf[:, sub, :], identity=ident[:]),
                     ["ybf", "ident"], ["pst"])
            y = ysb[qt % 2]
            yk = ("ysb", qt % 2)
            C.copy("act", y[:], pst[:].rearrange("p a b -> p (a b)"), ["pst"], [yk])
            C.load(yT[384:512, qsl], y[:], [yk], [("yTd", "c", qt)])
        return C.end_phase()


def mb_consts():
    t = {}
    t["mask64"] = (np.arange(64)[:, None] <= np.arange(64)[None, :]).astype(np.float32)
    t["ones64"] = np.ones((64, 128), np.float32)
    t["ident_f32"] = np.eye(128, dtype=np.float32)
    return t


def phase_MB(C, tag, n_grp=16):
    nc, S = C.nc, C.S
    sc = scratch_P(C, tag)
    mask_d = C.dram("mask64", [64, 64], F32)
    ones_d = C.dram("ones64", [64, 128], F32)
    idf_d = C.dram("ident_f32", [128, 128], F32)
    idb_d = C.dram("ident_bf", [128, 128], BF16)
    ifb_d = C.dram("ifb_" + tag, [128, 2], F32)
    gn_d = C.dram("gn_" + tag, [64, 256], F32)
    yT = C.dram("yT_" + tag, [512, T], BF16)
    with contextlib.ExitStack() as st:
        def sb(name, shape, dt):
            return st.enter_context(nc.sbuf_tensor("sb_" + name + "_mb" + tag, shape, dt))
        mask = sb("mask", [64, 64], F32)
        ones = sb("ones", [64, 128], F32)
        idf = sb("idf", [128, 128], F32)
        idb = sb("idb", [128, 128], BF16)
        ifb = sb("ifb", [128, 2], F32)
        gn = sb("gn", [64, 256], F32)
        gi = sb("gi", [128, 64], F32)
        gf = sb("gf", [128, 64], F32)
        lfT = sb("lfT", [64, 128], F32)
        ipT = sb("ipT", [64, 128], F32)
        EB = sb("EB", [64, 128], F32)
        EV = sb("EV", [64, 128], F32)
        EL = sb("EL", [128, 128], F32)
        tmpg = sb("tmpg", [64, 128], F32)
        Dst = sb("Dst", [128, 2, 257], F32)
        Cbf = sb("Cbf", [128, 2, 257], BF16)
        qk = [sb("qk%d" % i, [128, 4, 512], BF16) for i in range(2)]
        vo = [sb("vo%d" % i, [64, 8, 512], BF16) for i in range(2)]
        zz = [sb("zz%d" % i, [64, 8, 256], BF16) for i in range(2)]
        ktok = sb("ktok", [64, 8, 256], BF16)
        vaug = [sb("vaug%d" % i, [64, 257], BF16) for i in range(2)]
        pT = [sb("pT%d" % i, [64, 64], BF16) for i in range(2)]
        hbuf = sb("hbuf", [64, 8, 257], F32)
        dn = sb("dn", [64, 8], F32)
        rd = sb("rd", [64, 8], F32)
        h1 = sb("h1", [64, 8, 256], F32)
        h2 = sb("h2", [64, 8, 256], F32)
        ssq = sb("ssq", [64, 8], F32)
        rstd = sb("rstd", [64, 8], F32)
        yb = sb("yb", [64, 8, 256], BF16)
        ysb = sb("ysb", [128, 2, 512], BF16)
        psA = st.enter_context(nc.psum_tensor("psA_mb" + tag, [128, 512], F32))
        psB = st.enter_context(nc.psum_tensor("psB_mb" + tag, [128, 512], F32))
        pss = st.enter_context(nc.psum_tensor("pss_mb" + tag, [64, 64], F32))
        psh = st.enter_context(nc.psum_tensor("psh_mb" + tag, [64, 257], F32))
        psd = [st.enter_context(nc.psum_tensor("psd%d_mb%s" % (i, tag), [128, 257], F32)) for i in range(2)]
        pstk = st.enter_context(nc.psum_tensor("pstk_mb" + tag, [64, 8, 128], BF16))
        pty = st.enter_context(nc.psum_tensor("pty_mb" + tag, [128, 2, 8, 64], BF16))
        for (t_, d_, k_) in ((mask, mask_d, "mask"), (ones, ones_d, "ones"), (idf, idf_d, "idf"), (idb, idb_d, "idb"),
                             (ifb, ifb_d, "ifb"), (gn, gn_d, "gn")):
            C.load(t_[:], d_, [], [k_])
        C.load(gi[:], sc["gif"][0].rearrange("(j p) -> j p", p=64), [("gifd",)], ["gi"])
        C.load(gf[:], sc["gif"][1].rearrange("(j p) -> j p", p=64), [("gifd",)], ["gf"])
        S.op("dve", lambda e: e.tensor_scalar(out=gi[:], in0=gi[:], scalar1=ifb[:, 0:1], scalar2=None, op0=ALU.add),
             ["gi", "ifb"], ["gi"])
        S.op("dve", lambda e: e.tensor_scalar(out=gf[:], in0=gf[:], scalar1=ifb[:, 1:2], scalar2=-1.0,
                                              op0=ALU.add, op1=ALU.mult), ["gf", "ifb"], ["gf"])
        S.op("act", lambda e: e.activation(out=gf[:], in_=gf[:], func=AF.Exp), ["gf"], ["gf"])
        S.op("act", lambda e: e.activation(out=gf[:], in_=gf[:], func=AF.Ln, bias=1.0), ["gf"], ["gf"])
        S.op("pe", lambda e: e.transpose(out=psA[0:64, 0:128], in_=gf[:], identity=idf[:]), ["gf", "idf"], ["psA"])
        C.copy("dve", lfT[:], psA[0:64, 0:128], ["psA"], ["lfT"])
        S.op("pe", lambda e: e.transpose(out=psA[0:64, 0:128], in_=gi[:], identity=idf[:]), ["gi", "idf"], ["psA"])
        C.copy("dve", ipT[:], psA[0:64, 0:128], ["psA"], ["ipT"])
        C.mm(psA[0:64, 0:128], mask[:], lfT[:], True, True, ["mask", "lfT"], ["psA"])
        C.mm(psB[:, 0:128], ones[:], lfT[:], True, True, ["ones", "lfT"], ["psB"])
        S.op("act", lambda e: e.activation(out=EB[:], in_=psA[0:64, 0:128], func=AF.Exp, scale=-1.0), ["psA"], ["EB"])
        S.op("dve", lambda e: e.tensor_tensor(out=tmpg[:], in0=psA[0:64, 0:128], in1=ipT[:], op=ALU.add),
             ["psA", "ipT"], ["tmpg"])
        S.op("act", lambda e: e.activation(out=EV[:], in_=tmpg[:], func=AF.Exp), ["tmpg"], ["EV"])
        S.op("act", lambda e: e.activation(out=EL[:], in_=psB[:, 0:128], func=AF.Exp, scale=-1.0), ["psB"], ["EL"])
        t2v = sc["t2"].rearrange("(j p) c -> p j c", p=64)
        t3v = sc["t3"].rearrange("(j p) c -> p j c", p=64)
        bqv = sc["bqk"].rearrange("c p t -> p c t")
        for g in range(n_grp):
            gsl = slice(g * 512, (g + 1) * 512)
            q_ = qk[g % 2]
            qkk = ("qk", g % 2)
            v_ = vo[g % 2]
            vk = ("vo", g % 2)
            z_ = zz[g % 2]
            zk = ("zz", g % 2)
            C.load(q_[:], bqv[:, :, gsl], [("bqkd",)], [qkk])
            C.load(v_[:], t2v[:, g * 8:(g + 1) * 8, :], [("t2d",)], [vk])
            C.load(z_[:], t3v[:, g * 8:(g + 1) * 8, :], [("t3d",)], [zk])
            for jj in range(8):
                csl = slice(jj * 64, (jj + 1) * 64)
                for dc in range(2):
                    S.op("pe", lambda e, jj=jj, dc=dc, csl=csl, q_=q_: e.transpose(
                        out=pstk[:, jj, :] if dc == 0 else pstk[:, jj, :], in_=q_[:, 2 + dc, csl], identity=idb[:]),
                        [qkk, "idb"], ["pstk"])
                    S.op("act", lambda e, jj=jj, dc=dc: e.activation(
                        out=ktok[:, jj, dc * 128:(dc + 1) * 128], in_=pstk[:, jj, :], func=AF.Copy, scale=0.0625),
                        ["pstk"], ["ktok"])
            for jj in range(8):
                j = g * 8 + jj
                csl = slice(jj * 64, (jj + 1) * 64)
                va = vaug[j % 2]
                vak = ("vaug", j % 2)
                S.op("dve", lambda e, va=va, v_=v_, jj=jj, j=j: e.tensor_scalar(
                    out=va[:, 0:256], in0=v_[:, jj, 0:256], scalar1=EV[:, j:j + 1], scalar2=None, op0=ALU.mult),
                    [vk, "EV"], [vak])
                S.op("dve", lambda e, va=va, j=j: e.tensor_copy(out=va[:, 256:257], in_=EV[:, j:j + 1]), ["EV"], [vak])
                for dc in range(2):
                    C.mm(pss[:], q_[:, 2 + dc, csl], q_[:, dc, csl], dc == 0, dc == 1, [qkk], ["pss"])
                p_ = pT[j % 2]
                pk_ = ("pT", j % 2)
                S.op("dve", lambda e, p_=p_: e.scalar_tensor_tensor(
                    out=p_[:], in0=pss[:], scalar=0.0625, in1=mask[:], op0=ALU.mult, op1=ALU.mult),
                    ["pss", "mask"], [pk_])
                if j > 0:
                    for dc in range(2):
                        C.mm(psh[:], q_[:, dc, csl], Cbf[:, dc, :], dc == 0, False, [qkk, "Cbf"], ["psh"])
                C.mm(psh[:], p_[:], va[:], j == 0, True, [pk_, vak], ["psh"])
                S.op("act", lambda e, jj=jj, j=j: e.activation(
                    out=hbuf[:, jj, :], in_=psh[:], func=AF.Copy, scale=EB[:, j:j + 1]), ["psh", "EB"], ["hbuf"])
                for dc in range(2):
                    C.mm(psd[dc][:], ktok[:, jj, dc * 128:(dc + 1) * 128], va[:], True, True, ["ktok", vak], [("psd", dc)])
                    if j == 0:
                        C.copy("dve", Dst[:, dc, :], psd[dc][:], [("psd", dc)], ["Dst"])
                    else:
                        S.op("dve", lambda e, dc=dc, j=j: e.scalar_tensor_tensor(
                            out=Dst[:, dc, :], in0=Dst[:, dc, :], scalar=EL[:, j - 1:j], in1=psd[dc][:],
                            op0=ALU.mult, op1=ALU.add), [("psd", dc), "Dst", "EL"], ["Dst"])
                S.op("act", lambda e, j=j: e.activation(out=Cbf[:], in_=Dst[:], func=AF.Copy, scale=EL[:, j:j + 1]),
                     ["Dst", "EL"], ["Cbf"])
            S.op("dve", lambda e: e.scalar_tensor_tensor(out=dn[:], in0=hbuf[:, :, 256], scalar=-1.0, in1=hbuf[:, :, 256],
                                                         op0=ALU.mult, op1=ALU.max), ["hbuf"], ["dn"])
            S.op("dve", lambda e: e.tensor_scalar(out=dn[:], in0=dn[:], scalar1=1.0, scalar2=None, op0=ALU.max),
                 ["dn"], ["dn"])
            S.op("dve", lambda e: e.reciprocal(out=rd[:], in_=dn[:]), ["dn"], ["rd"])
            S.op("pool", lambda e: e.tensor_tensor(out=h1[:], in0=hbuf[:, :, 0:256],
                                                   in1=rd[:].unsqueeze(2).broadcast_to([64, 8, 256]), op=ALU.mult),
                 ["hbuf", "rd"], ["h1"])
            S.op("pool", lambda e, v_=v_: e.tensor_tensor(out=h2[:], in0=h1[:], in1=v_[:, :, 256:512], op=ALU.mult),
                 ["h1", vk], ["h2"])
            S.op("pool", lambda e: e.tensor_tensor(out=h1[:], in0=h2[:], in1=h2[:], op=ALU.mult), ["h2"], ["h1"])
            S.op("dve", lambda e: e.tensor_reduce(out=ssq[:], in_=h1[:], axis=AX.X, op=ALU.add), ["h1"], ["ssq"])
            S.op("act", lambda e: e.activation(out=ssq[:], in_=ssq[:], func=AF.Sqrt, bias=EPS, scale=1.0 / 256.0),
                 ["ssq"], ["ssq"])
            S.op("dve", lambda e: e.reciprocal(out=rstd[:], in_=ssq[:]), ["ssq"], ["rstd"])
            S.op("pool", lambda e: e.tensor_tensor(out=h1[:], in0=h2[:],
                                                   in1=rstd[:].unsqueeze(2).broadcast_to([64, 8, 256]), op=ALU.mult),
                 ["h2", "rstd"], ["h1"])
            S.op("pool", lambda e: e.tensor_tensor(out=h2[:], in0=h1[:],
                                                   in1=gn[:].unsqueeze(1).broadcast_to([64, 8, 256]), op=ALU.mult),
                 ["h1", "gn"], ["h2"])
            S.op("pool", lambda e, z_=z_: e.tensor_tensor(out=yb[:], in0=h2[:], in1=z_[:], op=ALU.mult),
                 ["h2", zk], ["yb"])
            for jj in range(8):
                for dc in range(2):
                    S.op("pe", lambda e, jj=jj, dc=dc: e.transpose(
                        out=pty[:, dc, jj, :], in_=yb[:, jj, dc * 128:(dc + 1) * 128], identity=idb[0:64, 0:64]),
                        ["yb", "idb"], ["pty"])
            C.copy("act", ysb[:].rearrange("p a t -> p (a t)"), pty[:].rearrange("p a j t -> p (a j t)"), ["pty"], ["ysb"])
            C.load(yT[128:384, gsl].rearrange("(a p) t -> p a t", p=128), ysb[:], ["ysb"], [("yTd", "b", g)])
        return C.end_phase()


def ma_consts():
    t = {}
    E = np.zeros((128, 64, 128), np.float32)
    for kt in range(64):
        E[2 * kt, kt, 0:64] = -NEG
        E[2 * kt + 1, kt, 64:128] = -NEG
    t["eall"] = E.astype(NPBF)
    OV = np.zeros((128, 4, 128), np.float32)
    for it in range(4):
        for il in range(128):
            i = 128 * it + il
            for j in range(128):
                if 4 * j - 1 <= i <= 4 * j + 3:
                    OV[il, it, j] = 1.0
    t["ov"] = OV.astype(NPBF)
    jj = np.broadcast_to(np.arange(128, dtype=np.float32)[None, :], (128, 128)).copy()
    t["jj"] = jj
    jj1 = jj.copy()
    jj1[:, 0] = 1.0e9
    t["jj1"] = jj1
    cv = np.zeros((128, 64), np.float32)
    for qs in range(64):
        cv[:, qs] = 2 * qs + (np.arange(128) >= 64) - 2
    t["cv"] = cv
    t["cv2"] = cv + 2.0
    return t


def prep_cmp(w1, pe, w2):
    w1a = np.ascontiguousarray(w1.reshape(16, 128, 64).transpose(1, 0, 2))
    pea = np.ascontiguousarray(pe.reshape(16, 128).T)
    return w1a.astype(np.float32), pea.astype(np.float32), np.ascontiguousarray(w2, dtype=np.float32)


def phase_MA(C, tag, n_qt=16):
    nc, S = C.nc, C.S
    sc = scratch_P(C, tag)
    qaug_d = C.dram("qaug", [2, 4, T], BF16)
    kaug_d = C.dram("kaug", [4, T], BF16)
    ident_d = C.dram("ident_bf", [128, 128], BF16)
    tri_d = C.dram("tri", [128, 4, 512], BF16)
    low_d = C.dram("low", [128, 4, 512], BF16)
    vis_d = C.dram("vis", [128, 5, 512], BF16)
    eall_d = C.dram("eall", [128, 64, 128], BF16)
    ov_d = C.dram("ov", [128, 4, 128], BF16)
    jj_d = C.dram("jj", [128, 128], F32)
    jj1_d = C.dram("jj1", [128, 128], F32)
    cv_d = C.dram("cv", [128, 64], F32)
    cv2_d = C.dram("cv2", [128, 64], F32)
    w1_d = [C.dram("w1%s_%s" % (s_, tag), [128, 16, 64], F32) for s_ in "kv"]
    pe_d = [C.dram("pe%s_%s" % (s_, tag), [128, 16], F32) for s_ in "kv"]
    w2_d = [C.dram("w2%s_%s" % (s_, tag), [64, 64], F32) for s_ in "kv"]
    yT = C.dram("yT_" + tag, [512, T], BF16)
    with contextlib.ExitStack() as st:
        def sb(name, shape, dt, stack=st):
            return stack.enter_context(nc.sbuf_tensor("sb_" + name + "_ma" + tag, shape, dt))
        ksa = sb("ksa", [68, T], BF16)
        kwa = sb("kwa", [68, T], BF16)
        vsa = sb("vsa", [128, 64, 65], BF16)
        vwa = sb("vwa", [128, 64, 65], BF16)
        ident = sb("ident", [128, 128], BF16)
        tri = sb("tri", [128, 4, 512], BF16)
        low = sb("low", [128, 4, 512], BF16)
        vis = sb("vis", [128, 5, 512], BF16)
        eall = sb("eall", [128, 64, 128], BF16)
        jj = sb("jj", [128, 128], F32)
        jj1 = sb("jj1", [128, 128], F32)
        cv = sb("cv", [128, 64], F32)
        cv2 = sb("cv2", [128, 64], F32)
        kcmpT = sb("kcmpT", [64, 512], BF16)
        vext = sb("vext", [128, 4, 193], BF16)
        pss = [st.enter_context(nc.psum_tensor("pss%d_ma%s" % (i, tag), [128, 512], F32)) for i in range(3)]
        pc = [st.enter_context(nc.psum_tensor("pc%d_ma%s" % (i, tag), [128, 2, 193], F32)) for i in range(2)]
        pso = [st.enter_context(nc.psum_tensor("pso%d_ma%s" % (i, tag), [128, 4, 65], F32)) for i in range(2)]
        pst = st.enter_context(nc.psum_tensor("pst_ma%s" % tag, [128, 4, 128], BF16))
        for (t_, d_, k_) in ((ident, ident_d, "ident"), (tri, tri_d, "tri"), (low, low_d, "low"), (vis, vis_d, "vis"),
                             (eall, eall_d, "eall"), (jj, jj_d, "jj"), (jj1, jj1_d, "jj1"), (cv, cv_d, "cv"), (cv2, cv2_d, "cv2")):
            C.load(t_[:], d_, [], [k_])
        C.load(ksa[0:64, :], sc["kskw"][0], [("kskwd",)], ["ksa"])
        C.load(ksa[64:68, :], kaug_d, [], ["ksa"])
        C.load(kwa[0:64, :], sc["kskw"][1], [("kskwd",)], ["kwa"])
        C.load(kwa[64:68, :], kaug_d, [], ["kwa"])
        C.load(vsa[:], sc["vst"][:, 0], [("vstd",)], ["vsa"])
        C.load(vwa[:], sc["vst"][:, 1], [("vstd",)], ["vwa"])
        S.op("pool", lambda e: e.memset(vext[:], 1.0), [], ["vext"])
        C.load(vext[:, :, 0:128], ov_d, ["vext"], ["vext"])
        with contextlib.ExitStack() as st2:
            kc2 = sb("kc2", [128, T + 16], BF16, st2)
            w1f = sb("w1f", [128, 16, 64], F32, st2)
            w1b = sb("w1b", [128, 16, 64], BF16, st2)
            pef = sb("pef", [128, 16], F32, st2)
            peb = sb("peb", [128, 16], BF16, st2)
            w2f = sb("w2f", [64, 64], F32, st2)
            w2b = sb("w2b", [64, 64], BF16, st2)
            bia = sb("bia", [64, 1], F32, st2)
            sgc = sb("sgc", [64, 512], F32, st2)
            hid = sb("hid", [64, 512], BF16, st2)
            for br in range(2):
                C.load(kc2[0:64, :], sc["kcvc"][br][:, 0:T + 16], [("kcvcd",)], ["kc2"])
                C.load(kc2[64:128, :], sc["kcvc"][br][:, 1:T + 17], [("kcvcd",)], ["kc2"])
                C.load(w1f[:], w1_d[br], [], ["w1f"])
                C.load(pef[:], pe_d[br], [], ["pef"])
                C.load(w2f[:], w2_d[br], [], ["w2f"])
                C.copy("dve", w1b[:], w1f[:], ["w1f"], ["w1b"])
                C.copy("dve", peb[:], pef[:], ["pef"], ["peb"])
                C.copy("dve", w2b[:], w2f[:], ["w2f"], ["w2b"])
                for c in range(16):
                    C.mm(pss[1][0:64, 0:1], w1b[:, c, :], peb[:, c:c + 1], c == 0, c == 15, ["w1b", "peb"], [("pss", 1)])
                C.copy("dve", bia[:], pss[1][0:64, 0:1], [("pss", 1)], ["bia"])
                for c in range(16):
                    C.mm(pss[0][0:64, :], w1b[:, c, :], kc2[:, 2 * c:2 * c + 16 * 511 + 1:16], c == 0, c == 15,
                         ["w1b", "kc2"], [("pss", 0)])
                S.op("act", lambda e: e.activation(out=sgc[:], in_=pss[0][0:64, :], func=AF.Sigmoid, bias=bia[:, 0:1]),
                     [("pss", 0), "bia"], ["sgc"])
                S.op("dve", lambda e: e.scalar_tensor_tensor(out=hid[:], in0=pss[0][0:64, :], scalar=bia[:, 0:1], in1=sgc[:],
                                                             op0=ALU.add, op1=ALU.mult), [("pss", 0), "bia", "sgc"], ["hid"])
                if br == 0:
                    C.mm(pss[2][0:64, :], w2b[:], hid[:], True, True, ["w2b", "hid"], [("pss", 2)])
                    C.copy("act", kcmpT[:], pss[2][0:64, :], [("pss", 2)], ["kcmpT"])
                else:
                    for it in range(4):
                        C.mm(pss[2][:, it * 64:(it + 1) * 64], hid[:, it * 128:(it + 1) * 128], w2b[:], it == 0, it == 3,
                             ["hid", "w2b"], [("pss", 2)])
                    C.copy("act", vext[:, :, 128:192], pss[2][:, 0:256].rearrange("p (a c) -> p a c", a=4),
                           [("pss", 2)], ["vext"])
            C.end_phase()
        qo = [[sb("qo%d_%d" % (i, h), [68, 512], BF16) for h in range(2)] for i in range(2)]
        qx = [[sb("qx%d_%d" % (i, h), [64, 512], BF16) for h in range(2)] for i in range(2)]
        gt = [sb("gt%d" % i, [128, 4, 6], F32) for i in range(2)]
        zt = [sb("zt%d" % i, [128, 4, 128], BF16) for i in range(2)]
        PT = [sb("PT%d" % i, [128, 512], BF16) for i in range(3)]
        U = [sb("U%d" % i, [128, 4, 193], F32) for i in range(4)]
        Us = [sb("Us%d" % i, [128, 4, 65], F32) for i in range(2)]
        Uw = [sb("Uw%d" % i, [128, 4, 65], F32) for i in range(2)]
        rdc = sb("rdc", [128, 4, 4], F32)
        imp = sb("imp", [128, 128], F32)
        elig = sb("elig", [128, 128], F32)
        impm = sb("impm", [128, 128], F32)
        impr = sb("impr", [128, 128], F32)
        Ft = sb("Ft", [128, 128], F32)
        m8a = sb("m8a", [128, 8], F32)
        m8b = sb("m8b", [128, 8], F32)
        nmb = sb("nmb", [128, 128], BF16)
        nmT = sb("nmT", [128, 512], BF16)
        rds = sb("rds", [128, 3, 4], F32)
        acc = sb("acc", [128, 4, 64], F32)
        tmp = sb("tmp", [128, 4, 64], F32)
        ybf = sb("ybf", [128, 4, 128], BF16)
        ysb = [sb("ysb%d" % i, [128, 512], BF16) for i in range(2)]
        st_ = dict(si=0)

        def attn(q, qk, K_, Kkey, kdim, V_, Vkey, kts, po, pok, vcols):
            for idx, (kt, bias, use_sel, vrow) in enumerate(kts):
                si = st_["si"]
                st_["si"] += 1
                ps, pk = pss[si % 3], ("pss", si % 3)
                pt, ptk = PT[si % 3], ("PT", si % 3)
                last_mm = "qk"
                n_extra = (bias is not None) + (1 if use_sel else 0)
                C.mm(ps[:], K_[0:kdim, kt * 128:(kt + 1) * 128], q[0:kdim, :], True, n_extra == 0, [Kkey, qk], [pk])
                if bias is not None:
                    C.mm(ps[:], ident[:], bias, False, not use_sel, ["ident", "tri", "low", "vis"], [pk])
                if use_sel:
                    C.mm(ps[:], eall[:, kt, :], nmT[:], False, True, ["eall", "nmT"], [pk])
                S.op("act", lambda e, ps=ps, pt=pt: e.activation(out=pt[:], in_=ps[:], func=AF.Exp, scale=0.125),
                     [pk], [ptk])
                for sub in range(4):
                    if vcols == 193:
                        o_ap = po[sub // 2][:, sub % 2, :]
                        okey = pok[sub // 2]
                        first = (idx == 0 and sub % 2 == 0)
                    else:
                        o_ap = po[:, sub, :]
                        okey = pok
                        first = (idx == 0 and sub == 0)
                    C.mm(o_ap, pt[:, sub * 128:(sub + 1) * 128], V_[:, vrow, :], first, idx == len(kts) - 1,
                         [ptk, Vkey], [okey])

        for qt in range(n_qt):
            qsl = slice(qt * 512, (qt + 1) * 512)
            g_, gk = gt[qt % 2], ("gt", qt % 2)
            z, zk = zt[qt % 2], ("zt", qt % 2)
            C.load(g_[:], sc["gst"][:, 4 * qt:4 * qt + 4, :], [("gstd",)], [gk])
            C.load(z[:], sc["zAC"][:, 4 * qt:4 * qt + 4, 0:128], [("zACd",)], [zk])
            qs_ = []
            for h in range(2):
                q, qk = qo[qt % 2][h], ("qo", qt % 2, h)
                C.load(q[0:64, :], sc["qA"][h][:, qsl], [("qAd",)], [qk])
                C.load(q[64:68, :], qaug_d[h][:, qsl], [], [qk])
                qs_.append((q, qk))
            for h in range(2):
                q, qk = qx[qt % 2][h], ("qx", qt % 2, h)
                C.load(q[:], sc["qA"][2 + h][:, qsl], [("qAd",)], [qk])
                qs_.append((q, qk))
            its = list(range(qt // 4 + 1))
            for hh in range(4):
                q, qk = qs_[hh]
                kts = []
                for it in its:
                    delta = 512 * qt - 2048 * it
                    kts.append((it, vis[:, delta // 512, :] if delta <= 2048 else None, False, it))
                attn(q, qk, kcmpT, "kcmpT", 64, vext, "vext", kts, pc, [("pc", 0), ("pc", 1)], 193)
                for half in range(2):
                    C.copy("dve" if half == 0 else "act", U[hh][:, 2 * half:2 * half + 2, :], pc[half][:],
                           [("pc", half)], [("U", hh)])
                S.op("dve", lambda e, hh=hh: e.tensor_scalar(out=rdc[:, hh, :], in0=U[hh][:, :, 192], scalar1=1.0e-30,
                                                             scalar2=None, op0=ALU.max), [("U", hh)], ["rdc"])
            S.op("dve", lambda e: e.reciprocal(out=rdc[:], in_=rdc[:]), ["rdc"], ["rdc"])
            if qt >= 2:
                for sub in range(4):
                    qs = 4 * qt + sub
                    S.op("dve", lambda e, sub=sub: e.tensor_scalar(out=imp[:], in0=U[0][:, sub, 0:128],
                                                                   scalar1=rdc[:, 0, sub:sub + 1], scalar2=None, op0=ALU.mult),
                         [("U", 0), "rdc"], ["imp"])
                    for hh in range(1, 4):
                        S.op("dve", lambda e, sub=sub, hh=hh: e.scalar_tensor_tensor(
                            out=imp[:], in0=U[hh][:, sub, 0:128], scalar=rdc[:, hh, sub:sub + 1], in1=imp[:],
                            op0=ALU.mult, op1=ALU.add), [("U", hh), "rdc", "imp"], ["imp"])
                    S.op("dve", lambda e, qs=qs: e.tensor_scalar(out=elig[:], in0=jj1[:], scalar1=cv[:, qs:qs + 1],
                                                                 scalar2=None, op0=ALU.is_le), ["jj1", "cv"], ["elig"])
                    S.op("dve", lambda e: e.tensor_tensor(out=impm[:], in0=imp[:], in1=elig[:], op=ALU.mult),
                         ["imp", "elig"], ["impm"])
                    S.op("dve", lambda e: e.max(out=m8a[:], in_=impm[:]), ["impm"], ["m8a"])
                    S.op("dve", lambda e: e.match_replace(out=impr[:], in_to_replace=m8a[:], in_values=impm[:],
                                                          imm_value=-1.0), ["m8a", "impm"], ["impr"])
                    S.op("dve", lambda e: e.max(out=m8b[:], in_=impr[:]), ["impr"], ["m8b"])
                    S.op("dve", lambda e, qs=qs: e.tensor_scalar(out=Ft[:], in0=jj[:], scalar1=cv2[:, qs:qs + 1],
                                                                 scalar2=-1.0, op0=ALU.is_le, op1=ALU.add),
                         ["jj", "cv2"], ["Ft"])
                    S.op("dve", lambda e: e.tensor_tensor(out=Ft[:], in0=Ft[:], in1=elig[:], op=ALU.subtract),
                         ["Ft", "elig"], ["Ft"])
                    S.op("dve", lambda e: e.scalar_tensor_tensor(out=nmb[:], in0=impm[:], scalar=m8b[:, 4:5], in1=Ft[:],
                                                                 op0=ALU.is_ge, op1=ALU.add), ["impm", "m8b", "Ft"], ["nmb"])
                    S.op("pe", lambda e, sub=sub: e.transpose(out=pst[:, sub, :], in_=nmb[:], identity=ident[:]),
                         ["nmb", "ident"], ["pst"])
                C.copy("act", nmT[:], pst[:].rearrange("p a b -> p (a b)"), ["pst"], ["nmT"])
            for h in range(2):
                q, qk = qs_[h]
                kts = [(kt, tri[:, kt - 4 * qt, :] if kt >= 4 * qt else None, qt >= 2, kt) for kt in range(4 * qt + 4)]
                attn(q, qk, ksa, "ksa", 68, vsa, "vsa", kts, pso[0], ("pso", 0), 65)
                C.copy("dve", Us[h][:], pso[0][:], [("pso", 0)], [("Us", h)])
                kts = []
                for kt in range(max(0, 4 * qt - 4), 4 * qt + 4):
                    b_ = low[:, kt - (4 * qt - 4), :] if kt < 4 * qt else tri[:, kt - 4 * qt, :]
                    kts.append((kt, b_, False, kt))
                attn(q, qk, kwa, "kwa", 68, vwa, "vwa", kts, pso[1], ("pso", 1), 65)
                C.copy("dve", Uw[h][:], pso[1][:], [("pso", 1)], [("Uw", h)])
            for h in range(2):
                S.op("dve", lambda e, h=h: e.tensor_copy(out=rds[:, 0, :], in_=rdc[:, h, :]), ["rdc"], ["rds"])
                S.op("dve", lambda e, h=h: e.reciprocal(out=rds[:, 1, :], in_=Us[h][:, :, 64]), [("Us", h)], ["rds"])
                S.op("dve", lambda e, h=h: e.reciprocal(out=rds[:, 2, :], in_=Uw[h][:, :, 64]), [("Uw", h)], ["rds"])
                S.op("dve", lambda e, h=h, g_=g_: e.tensor_tensor(
                    out=rds[:], in0=rds[:], in1=g_[:, :, 3 * h:3 * h + 3].rearrange("p s b -> p b s"), op=ALU.mult),
                    ["rds", gk], ["rds"])
                srcs = [(U[h][:, :, 128:192], ("U", h)), (Us[h][:, :, 0:64], ("Us", h)), (Uw[h][:, :, 0:64], ("Uw", h))]
                for bi, (src, skey) in enumerate(srcs):
                    dst, dkey = (acc, "acc") if bi == 0 else (tmp, "tmp")
                    S.op("pool", lambda e, src=src, dst=dst, bi=bi: e.tensor_tensor(
                        out=dst[:], in0=src, in1=rds[:, bi, :].unsqueeze(2).broadcast_to([128, 4, 64]), op=ALU.mult),
                        [skey, "rds"], [dkey])
                    if bi > 0:
                        S.op("pool", lambda e: e.tensor_tensor(out=acc[:], in0=acc[:], in1=tmp[:], op=ALU.add),
                             ["acc", "tmp"], ["acc"])
                S.op("pool", lambda e, h=h, z=z: e.tensor_tensor(
                    out=ybf[:, :, h * 64:(h + 1) * 64], in0=acc[:], in1=z[:, :, h * 64:(h + 1) * 64], op=ALU.mult),
                    ["acc", zk], ["ybf"])
            for sub in range(4):
                S.op("pe", lambda e, sub=sub: e.transpose(out=pst[:, sub, :], in_=ybf[:, sub, :], identity=ident[:]),
                     ["ybf", "ident"], ["pst"])
            y, yk = ysb[qt % 2], ("ysb", qt % 2)
            C.copy("act", y[:], pst[:].rearrange("p a b -> p (a b)"), ["pst"], [yk])
            C.load(yT[0:128, qsl], y[:], [yk], [("yTd", "a", qt)])
        return C.end_phase()


def _garr(g):
    return np.ascontiguousarray(np.asarray(g, np.float32).reshape(16, 128).T)


def _wout_perm(w_out):
    idx = []
    for r in range(4):
        idx += _rng(128 * r, 128) + _rng(512 + 256 * r, 256) + _rng(1536 + 128 * r, 128)
    return np.ascontiguousarray(w_out[np.array(idx), :], dtype=np.float32)


def _launch(io, build, maps):
    nc = bass.Bass("TRN2", target_bir_lowering=False)
    with contextlib.ExitStack() as st:
        C = Ctx(nc, st, io)
        build(C)
    res = run_bass_kernel_spmd(nc, maps, core_ids=list(range(8)))
    return res.results


def _mixer_launch(inp, l, h_parts):
    tag = "l%d" % l
    ct = const_tables()
    ma = ma_consts()
    mc = mb_consts()
    cn = ["kaug", "ident_bf", "band", "tri", "low", "vis", "eall", "ov", "jj", "jj1", "cv", "cv2",
          "mask64", "ones64", "ident_f32"]
    allc = {}
    allc.update(ct)
    allc.update(ma)
    allc.update(mc)
    io = {"hg_" + tag: "in", "wpm_" + tag: "in", "cwb_" + tag: "in", "qaug": "in", "sinks_" + tag: "in",
          "ifb_" + tag: "in", "gn_" + tag: "in", "yT_" + tag: "out"}
    for n in cn:
        io[n] = "in"
    for n in ("w1k", "w1v", "pek", "pev", "w2k", "w2v"):
        io[n + "_" + tag] = "in"

    def build(C):
        phase_P(C, tag)
        phase_MC(C, tag)
        phase_MB(C, tag)
        phase_MA(C, tag)

    w1k, pek, w2k = prep_cmp(inp["cmp_w1_k"][l], inp["cmp_pe_k"][l], inp["cmp_w2_k"][l])
    w1v, pev, w2v = prep_cmp(inp["cmp_w1_v"][l], inp["cmp_pe_v"][l], inp["cmp_w2_v"][l])
    maps = []
    for c in range(8):
        b, hq = c // 4, c % 4
        m = {"hg_" + tag: np.stack([h_parts[b * 4 + r] for r in range(4)]),
             "wpm_" + tag: take_cols(np.asarray(inp["w_in"][l], np.float32), wpm_cols(hq)),
             "cwb_" + tag: prep_cwb(np.asarray(inp["mlstm_conv_w"][l], np.float32),
                                    np.asarray(inp["mlstm_conv_b"][l], np.float32), hq),
             "qaug": qaug_table(hq),
             "sinks_" + tag: np.broadcast_to(np.asarray(inp["swa_sinks"][l], np.float32)[[2 * hq, 2 * hq + 1]][None, :],
                                             (128, 2)).copy(),
             "ifb_" + tag: np.broadcast_to(np.array([inp["mlstm_i_bias"][l][hq], inp["mlstm_f_bias"][l][hq]],
                                                    np.float32)[None, :], (128, 2)).copy(),
             "gn_" + tag: np.broadcast_to(np.asarray(inp["mlstm_norm_g"][l], np.float32)[256 * hq:256 * hq + 256][None, :],
                                          (64, 256)).copy(),
             "w1k_" + tag: w1k, "pek_" + tag: pek, "w2k_" + tag: w2k,
             "w1v_" + tag: w1v, "pev_" + tag: pev, "w2v_" + tag: w2v}
        for n in cn:
            m[n] = allc[n]
        maps.append(m)
    res = _launch(io, build, maps)
    return [np.asarray(res[c]["yT_" + tag]) for c in range(8)]


def kernel(**inputs):
    inp = inputs
    x = np.asarray(inp["x"], np.float32)
    ones = np.ones((128, 128), np.float32)
    xr = [np.ascontiguousarray(x[c // 4, (c % 4) * TQ:(c % 4 + 1) * TQ, :].T) for c in range(8)]
    res = _launch({"xr0": "in", "g_n0": "in", "ones_f32": "in", "h0": "out"},
                  lambda C: phase_ON(C, "n0", False, False, "xr0", None, "h0"),
                  [{"xr0": xr[c], "g_n0": _garr(inp["norm_g"][0]), "ones_f32": ones} for c in range(8)])
    h_parts = [np.asarray(res[c]["h0"]) for c in range(8)]
    out = None
    for l in range(2):
        y = _mixer_launch(inp, l, h_parts)
        last = (l == 1)
        tag = "o%d" % l
        io = {"xin": "in", "g_" + tag: "in", "ones_f32": "in", "mixo_" + tag: "in", "wout_" + tag: "in"}
        if last:
            io["outT"] = "out"
        else:
            io["xout"] = "out"
            io["hnext"] = "out"
        wp = _wout_perm(np.asarray(inp["w_out"][l], np.float32))
        g = _garr(inp["final_norm_g"] if last else inp["norm_g"][l + 1])
        maps = []
        for c in range(8):
            b, r = c // 4, c % 4
            mixo = np.stack([y[b * 4 + rr][:, r * TQ:(r + 1) * TQ] for rr in range(4)])
            maps.append({"xin": xr[c], "g_" + tag: g, "ones_f32": ones, "mixo_" + tag: np.ascontiguousarray(mixo),
                         "wout_" + tag: wp})
        res = _launch(io, lambda C, tag=tag, last=last: phase_ON(C, tag, True, last, "xin", "xout", "hnext"), maps)
        if last:
            out = np.empty((2, T, D), np.float32)
            for c in range(8):
                out[c // 4, (c % 4) * TQ:(c % 4 + 1) * TQ, :] = np.asarray(res[c]["outT"]).T
        else:
            xr = [np.asarray(res[c]["xout"]) for c in range(8)]
            h_parts = [np.asarray(res[c]["hnext"]) for c in range(8)]
    return out
```
